# Optimizing a Trainium2 kernel written in Bass

```python
import jax, jax.numpy as jnp
from jax import lax
import numpy as np

D_MODEL = 1024
BATCH = 8
SEQ = 8192
DEPTH = 2

D_MIX = D_MODEL
D_POOL = D_MIX // 2
POOL_WINDOWS = (2, 4, 8, 16)
N_POOL_GROUPS = len(POOL_WINDOWS)
POOL_GROUP = D_POOL // N_POOL_GROUPS
D_ATTN = D_MIX - D_POOL
N_HEADS = 8
HEAD_DIM = D_ATTN // N_HEADS
ROT_DIM = HEAD_DIM // 4
ROPE_THETA = 500000.0
N_IDX_HEADS = 8
IDX_DIM = 64
IDX_ROT = IDX_DIM // 4
TOPK_MAX = 256
Q_BLOCK = 128
N_KEYS = 128
N_EXPERTS = N_KEYS * N_KEYS
PEER_HEADS = 8
PEER_QDIM = 256
PEER_HALF = PEER_QDIM // 2
PEER_TOPK = 16
PEER_CHUNK = 128
EPS = 1e-6
IN_SPLITS = (D_POOL, D_ATTN, D_ATTN, D_ATTN, N_IDX_HEADS * IDX_DIM, IDX_DIM, N_IDX_HEADS)
D_IN = sum(IN_SPLITS)

kernel_name = "hybrid_pool_dsa_peer_adaln"


def rms_norm(x, g):
    xf = x.astype(jnp.float32)
    y = xf * lax.rsqrt(jnp.mean(xf * xf, axis=-1, keepdims=True) + EPS)
    return (y * g.astype(jnp.float32)).astype(x.dtype)


def rope_tables(positions, rot_dim):
    inv = ROPE_THETA ** (-jnp.arange(0, rot_dim, 2, dtype=jnp.float32) / rot_dim)
    ang = positions.astype(jnp.float32)[..., None] * inv
    return jnp.cos(ang), jnp.sin(ang)


def rope_partial(x, cos, sin):
    half = cos.shape[-1]
    rot = 2 * half
    xf = x.astype(jnp.float32)
    x1, x2, xp = xf[..., :half], xf[..., half:rot], xf[..., rot:]
    c, s = cos[:, :, None, :], sin[:, :, None, :]
    return jnp.concatenate([x1 * c - x2 * s, x1 * s + x2 * c, xp], axis=-1).astype(x.dtype)


def multiscale_pool(xp, w_pool, pool_scale):
    B, S, _ = xp.shape
    xf = xp.astype(jnp.float32).reshape(B, S, N_POOL_GROUPS, POOL_GROUP)
    cs = jnp.cumsum(xf, axis=1)
    t = jnp.arange(S)
    outs = []
    for g, w in enumerate(POOL_WINDOWS):
        cs_g = cs[:, :, g]
        lag = jnp.pad(cs_g, ((0, 0), (w, 0), (0, 0)))[:, :S]
        cnt = jnp.minimum(t + 1, w).astype(jnp.float32)[None, :, None]
        outs.append((cs_g - lag) / cnt - xf[:, :, g])
    pooled = jnp.stack(outs, axis=2).astype(xp.dtype)
    mixed = jnp.einsum('bsgc,gcd->bsgd', pooled, w_pool)
    return mixed.reshape(B, S, D_POOL) * pool_scale


def dsa_attention(q, k, v, q_idx, k_idx, w_idx, topk):
    B, S, H, hd = q.shape
    n_blocks = S // Q_BLOCK
    key_pos = jnp.arange(S)
    att_scale = HEAD_DIM ** -0.5
    kf = k_idx.astype(jnp.float32)

    def block(i):
        s0 = i * Q_BLOCK
        qb = lax.dynamic_slice_in_dim(q, s0, Q_BLOCK, axis=1)
        qib = lax.dynamic_slice_in_dim(q_idx, s0, Q_BLOCK, axis=1)
        wib = lax.dynamic_slice_in_dim(w_idx, s0, Q_BLOCK, axis=1)
        qpos = s0 + jnp.arange(Q_BLOCK)
        dots = jnp.einsum('bqhd,bsd->bqhs', qib.astype(jnp.float32), kf)
        score = jnp.einsum('bqhs,bqh->bqs', jax.nn.relu(dots), wib.astype(jnp.float32))
        causal = key_pos[None, :] <= qpos[:, None]
        score = jnp.where(causal[None], score, -jnp.inf)
        _, sel = lax.top_k(score, topk)
        kg = jax.vmap(lambda kk, ii: kk[ii])(k, sel)
        vg = jax.vmap(lambda vv, ii: vv[ii])(v, sel)
        logits = jnp.einsum('bqhd,bqkhd->bqhk', qb, kg).astype(jnp.float32) * att_scale
        valid = sel <= qpos[None, :, None]
        logits = jnp.where(valid[:, :, None, :], logits, -jnp.inf)
        p = jax.nn.softmax(logits, axis=-1).astype(v.dtype)
        return jnp.einsum('bqhk,bqkhd->bqhd', p, vg)

    out = lax.map(block, jnp.arange(n_blocks))
    return out.transpose(1, 0, 2, 3, 4).reshape(B, S, H * hd)


def peer_ffn(h, w_query, sub_keys, expert_u, expert_v):
    B, S, D = h.shape
    tokens = h.reshape(-1, PEER_CHUNK, D)

    def chunk(xc):
        T = xc.shape[0]
        q = (xc @ w_query).reshape(T, PEER_HEADS, 2, PEER_HALF)
        s = jnp.einsum('thpd,hpnd->thpn', q.astype(jnp.float32), sub_keys.astype(jnp.float32))
        sv, si = lax.top_k(s, PEER_TOPK)
        cand = (sv[:, :, 0, :, None] + sv[:, :, 1, None, :]).reshape(T, PEER_HEADS, -1)
        cand_idx = (si[:, :, 0, :, None] * N_KEYS + si[:, :, 1, None, :]).reshape(T, PEER_HEADS, -1)
        top_s, pos = lax.top_k(cand, PEER_TOPK)
        idx = jnp.take_along_axis(cand_idx, pos, axis=-1)
        g = jax.nn.softmax(top_s, axis=-1)
        u = expert_u[idx]
        v = expert_v[idx]
        a = jax.nn.gelu(jnp.einsum('td,thkd->thk', xc, u), approximate=False)
        return jnp.einsum('thk,thkd->td', (g * a).astype(v.dtype), v)

    return lax.map(chunk, tokens).reshape(B, S, D)


def setup_inputs(seed: int = 0) -> dict:
    key = jax.random.key(seed)
    ks = jax.random.split(key, 18)
    f32 = jnp.float32
    n = lambda k, shape, s: jax.random.normal(k, shape, f32) * s
    x = jax.random.normal(ks[0], (BATCH, SEQ, D_MODEL), f32)
    c = jax.random.normal(ks[1], (BATCH, D_MODEL), f32)
    offsets = jax.random.randint(ks[2], (BATCH, 1), 0, 4096, dtype=jnp.int32)
    positions = (offsets + jnp.arange(SEQ, dtype=jnp.int32)[None, :]).astype(jnp.int32)
    return {
        "x": x,
        "c": c,
        "positions": positions,
        "w_ada": n(ks[3], (DEPTH, D_MODEL, 6 * D_MODEL), 0.5 * D_MODEL ** -0.5),
        "b_ada": n(ks[4], (DEPTH, 6 * D_MODEL), 0.02),
        "norm_mix": 1.0 + n(ks[5], (DEPTH, D_MODEL), 0.02),
        "w_in": n(ks[6], (DEPTH, D_MODEL, D_IN), D_MODEL ** -0.5),
        "w_pool": n(ks[7], (DEPTH, N_POOL_GROUPS, POOL_GROUP, POOL_GROUP), POOL_GROUP ** -0.5),
        "pool_scale": 1.0 + n(ks[8], (DEPTH, D_POOL), 0.1),
        "w_out": n(ks[9], (DEPTH, D_MIX, D_MODEL), D_MIX ** -0.5),
        "norm_ffn": 1.0 + n(ks[10], (DEPTH, D_MODEL), 0.02),
        "w_query": n(ks[11], (DEPTH, D_MODEL, PEER_HEADS * PEER_QDIM), D_MODEL ** -0.5),
        "sub_keys": n(ks[12], (DEPTH, PEER_HEADS, 2, N_KEYS, PEER_HALF), PEER_HALF ** -0.5),
        "expert_u": n(ks[13], (DEPTH, N_EXPERTS, D_MODEL), D_MODEL ** -0.5),
        "expert_v": n(ks[14], (DEPTH, N_EXPERTS, D_MODEL), 0.5),
        "final_norm": 1.0 + n(ks[15], (D_MODEL,), 0.02),
    }


def reference(x, c, positions, w_ada, b_ada, norm_mix, w_in, w_pool, pool_scale, w_out,
              norm_ffn, w_query, sub_keys, expert_u, expert_v, final_norm):
    B, S, D = x.shape
    topk = min(TOPK_MAX, S // 4)
    cos, sin = rope_tables(positions, ROT_DIM)
    split_at = list(np.cumsum(IN_SPLITS)[:-1])
    for l in range(DEPTH):
        mod = (jax.nn.silu(c) @ w_ada[l] + b_ada[l])[:, None, :]
        sh1, sc1, g1, sh2, sc2, g2 = jnp.split(mod, 6, axis=-1)
        h = rms_norm(x, norm_mix[l]) * (1 + sc1) + sh1
        proj = h @ w_in[l]
        xp, q, k, v, q_idx, k_idx, w_idx = jnp.split(proj, split_at, axis=-1)
        pool_out = multiscale_pool(xp, w_pool[l], pool_scale[l])
        q = rope_partial(q.reshape(B, S, N_HEADS, HEAD_DIM), cos, sin)
        k = rope_partial(k.reshape(B, S, N_HEADS, HEAD_DIM), cos, sin)
        v = v.reshape(B, S, N_HEADS, HEAD_DIM)
        q_idx = rope_partial(q_idx.reshape(B, S, N_IDX_HEADS, IDX_DIM), cos, sin) * (IDX_DIM ** -0.5)
        k_idx = rope_partial(k_idx[:, :, None, :], cos, sin)[:, :, 0]
        w_idx = w_idx * (N_IDX_HEADS ** -0.5)
        attn_out = dsa_attention(q, k, v, q_idx, k_idx, w_idx, topk)
        mixed = jnp.concatenate([pool_out, attn_out], axis=-1)
        x = x + g1 * (mixed @ w_out[l])
        h = rms_norm(x, norm_ffn[l]) * (1 + sc2) + sh2
        x = x + g2 * peer_ffn(h, w_query[l], sub_keys[l], expert_u[l], expert_v[l])
    return rms_norm(x, final_norm)
```

```python
import numpy as np
from contextlib import ExitStack
import concourse.bass as bass
import concourse.mybir as mybir
from concourse.bass_utils import run_bass_kernel_spmd

F32 = mybir.dt.float32
BF16 = mybir.dt.bfloat16
I32 = mybir.dt.int32
AF = mybir.ActivationFunctionType
ALU = mybir.AluOpType
AX = mybir.AxisListType

D = 1024
DIN = 2632
NE = 16384
NEG = -1.0e30
NDMA_SEMS = 16
COMPUTE = ("pe", "act", "dve", "pool")


class Sched:
    def __init__(self):
        self.ops = []
        self.last_w = {}
        self.rd_eng = {}
        self.rd_dma = {}
        self.epoch = 0
        self.op_epoch = []
        self.is_barrier = []

    def add(self, eng, fn, r=(), w=(), dma=False):
        idx = len(self.ops)
        deps = set()
        if "__epoch" not in w:
            r = list(r) + ["__epoch"]
        w = list(w) + [x for x in r if isinstance(x, str) and x.startswith("P:")]
        for x in r:
            lw = self.last_w.get(x)
            if lw is not None:
                deps.add((lw, "raw"))
        for x in w:
            lw = self.last_w.get(x)
            if lw is not None:
                deps.add((lw, "waw"))
            for e2, oi in self.rd_eng.get(x, {}).items():
                deps.add((oi, "war"))
            for oi in self.rd_dma.get(x, ()):
                deps.add((oi, "war"))
        for x in r:
            if dma:
                self.rd_dma.setdefault(x, []).append(idx)
            else:
                self.rd_eng.setdefault(x, {})[eng] = idx
        for x in w:
            self.last_w[x] = idx
            self.rd_eng[x] = {}
            self.rd_dma[x] = []
        final = set()
        for (oi, kind) in deps:
            if oi == idx:
                continue
            oeng, _, _, odma = self.ops[oi]
            if (not odma) and (not dma) and oeng == eng == "pe":
                continue
            final.add(oi)
        self.ops.append((eng, fn, sorted(final), dma))
        self.op_epoch.append(self.epoch)
        isb = "__epoch" in w
        self.is_barrier.append(isb)
        if isb:
            self.epoch += 1
        return idx

    def emit(self, nc):
        ops = self.ops
        NSETS = 8
        ND = 8
        ep = self.op_epoch
        ordinal = [0] * len(ops)
        cnt = {}
        dcnt = {}
        dma_slot = [None] * len(ops)
        for i, (eng, fn, deps, dma) in enumerate(ops):
            sset = ep[i] % NSETS
            if dma:
                dma_slot[i] = dcnt.get(sset, 0)
                dcnt[sset] = dma_slot[i] + 1
            else:
                cnt[(sset, eng)] = cnt.get((sset, eng), 0) + 1
                ordinal[i] = cnt[(sset, eng)]
        by_eng = {}
        for i, o in enumerate(ops):
            by_eng.setdefault(o[0], []).append(i)

        with ExitStack() as st:
            csem = {(k, e): st.enter_context(nc.semaphore("s_%s%d" % (e, k))) for k in range(NSETS) for e in COMPUTE}
            dsem = {(k, j): st.enter_context(nc.semaphore("s_dma%d_%d" % (k, j))) for k in range(NSETS) for j in range(ND)}
            block = st.enter_context(nc.Block())

            def run(engname, e):
                seen = {}
                for i in by_eng.get(engname, []):
                    eng, fn, deps, dma = ops[i]
                    sset = ep[i] % NSETS
                    need = {}
                    for oi in deps:
                        if ep[oi] != ep[i] and not self.is_barrier[oi]:
                            continue
                        oeng, _, _, odma = ops[oi]
                        oset = ep[oi] % NSETS
                        if odma:
                            j = dma_slot[oi]
                            key = ("d", oset, j % ND)
                            val = 16 * (j // ND + 1)
                        else:
                            key = ("c", oset, oeng)
                            val = ordinal[oi]
                        if need.get(key, 0) < val:
                            need[key] = val
                    if dma:
                        j = dma_slot[i]
                        if j >= ND:
                            key = ("d", sset, j % ND)
                            val = 16 * (j // ND)
                            if need.get(key, 0) < val:
                                need[key] = val
                    for key, val in need.items():
                        if seen.get(key, 0) >= val:
                            continue
                        seen[key] = val
                        sem = dsem[(key[1], key[2])] if key[0] == "d" else csem[(key[1], key[2])]
                        e.wait_ge(sem, val)
                    ins = fn(e)
                    if dma:
                        ins.then_inc(dsem[(sset, dma_slot[i] % ND)], 16)
                    else:
                        ins.then_inc(csem[(sset, eng)], 1)
                if engname == "sp":
                    for k in range(NSETS):
                        tot = dcnt.get(k, 0)
                        for j in range(ND):
                            n_k = (tot - j + ND - 1) // ND if tot > j else 0
                            if n_k > 0:
                                e.wait_ge(dsem[(k, j)], 16 * n_k)

            @block.sync
            def _(e):
                run("sp", e)

            @block.tensor
            def _(e):
                run("pe", e)

            @block.scalar
            def _(e):
                run("act", e)

            @block.vector
            def _(e):
                run("dve", e)

            @block.gpsimd
            def _(e):
                run("pool", e)


class K:
    def __init__(self, S, L=2, dbg=False, stop_after=None):
        self.S = S
        self.NT = S // 128
        self.L = L
        self.dbg = dbg
        self.stop_after = stop_after
        self.topk = min(256, S // 4)
        self.nc = bass.Bass("TRN2", target_bir_lowering=False)
        self.sc = Sched()
        self.uid = 0

    def dram(self, name, shape, dt, kind="Internal"):
        if kind == "Internal" and self.dbg:
            kind = "ExternalOutput"
        return self.nc.dram_tensor(name, list(shape), dt, kind=kind).ap()

    def sb(self, st, name, shape, dt):
        self.uid += 1
        return st.enter_context(self.nc.sbuf_tensor("%s_%d" % (name, self.uid), list(shape), dt))

    def ps(self, st, name, shape, dt):
        self.uid += 1
        return st.enter_context(self.nc.psum_tensor("%s_%d" % (name, self.uid), list(shape), dt))

    def op(self, eng, fn, r=(), w=()):
        return self.sc.add(eng, fn, r, w, dma=False)

    def dma(self, out, in_, r=(), w=(), **kw):
        return self.sc.add("sp", lambda e: e.dma_start(out=out, in_=in_, **kw), r, w, dma=True)

    def barrier(self):
        d = self.dummy
        return self.op("pool", lambda e: e.memset(d[:], 0.0), r=(), w=["__epoch"])

    def copy(self, eng, out, in_, r, w):
        if eng == "act":
            return self.op("act", lambda e: e.activation(out=out, in_=in_, func=AF.Copy), r, w)
        return self.op(eng, lambda e: e.tensor_copy(out=out, in_=in_), r, w)

    def act(self, out, in_, func, r, w, bias=None, scale=None, accum_out=None):
        kw = {}
        if bias is not None:
            kw["bias"] = bias
        if scale is not None:
            kw["scale"] = scale
        if accum_out is not None:
            kw["accum_out"] = accum_out
        return self.op("act", lambda e: e.activation(out=out, in_=in_, func=func, **kw), r, w)

    def tt(self, eng, out, in0, in1, op, r, w):
        return self.op(eng, lambda e: e.tensor_tensor(out=out, in0=in0, in1=in1, op=op), r, w)

    def ts(self, eng, out, in0, s1, op0, r, w, s2=None, op1=None, accum_out=None):
        kw = {}
        if op1 is not None:
            kw["op1"] = op1
        if accum_out is not None:
            kw["accum_out"] = accum_out
        return self.op(eng, lambda e: e.tensor_scalar(out=out, in0=in0, scalar1=s1, scalar2=s2, op0=op0, **kw), r, w)

    def stt(self, out, in0, scalar, in1, op0, op1, r, w):
        return self.op("dve", lambda e: e.scalar_tensor_tensor(out=out, in0=in0, scalar=scalar, in1=in1, op0=op0, op1=op1), r, w)

    def mms(self, specs, r, w):
        def fn(e):
            ins = None
            for (o, l, rr, s0, s1) in specs:
                ins = e.matmul(o, lhsT=l, rhs=rr, start=s0, stop=s1)
            return ins
        return self.op("pe", fn, r, w)

    def transposes(self, specs, ident, r, w):
        def fn(e):
            ins = None
            for (o, i) in specs:
                ins = e.transpose(out=o, in_=i, identity=ident)
            return ins
        return self.op("pe", fn, r, w)

    def build(self):
        nc = self.nc
        S, NT, L = self.S, self.NT, self.L
        ein = "ExternalInput"
        self.x_d = self.dram("x", [S, D], F32, ein)
        self.c_d = self.dram("c", [128, 8], F32, ein)
        self.pos_d = self.dram("pos", [128, NT], I32, ein)
        self.w_ada = self.dram("w_ada", [L, D, 6 * D], F32, ein)
        self.b_ada = self.dram("b_ada", [L, 6 * D], F32, ein)
        self.norm_mix = self.dram("norm_mix", [L, D], F32, ein)
        self.w_in = self.dram("w_in", [L, D, DIN], F32, ein)
        self.w_pool = self.dram("w_pool", [L, 4, 128, 128], F32, ein)
        self.pool_scale = self.dram("pool_scale", [L, 512], F32, ein)
        self.w_out = self.dram("w_out", [L, D, D], F32, ein)
        self.norm_ffn = self.dram("norm_ffn", [L, D], F32, ein)
        self.w_query = self.dram("w_query", [L, D, 2048], F32, ein)
        self.sub_keys = self.dram("sub_keys", [L, 8, 2, 128, 128], F32, ein)
        self.expert_u = self.dram("expert_u", [L, NE, D], F32, ein)
        self.expert_v = self.dram("expert_v", [L, NE, D], F32, ein)
        self.final_norm = self.dram("final_norm", [1, D], F32, ein)
        self.out_d = self.dram("out", [S, D], F32, "ExternalOutput")
        self.xres_d = self.dram("xres", [S, D], F32)
        self.mixT_d = self.dram("mixT", [8, 128, S], BF16)
        self.qT_d = self.dram("qT", [4, 128, S], BF16)
        self.kT_d = self.dram("kT", [4, 128, S], BF16)
        self.qiT_d = self.dram("qiT", [4, 128, S], BF16)
        self.kiT_d = self.dram("kiT", [128, S], BF16)
        self.vaug_d = self.dram("vaug", [S, 520], BF16)
        self.h2T_d = self.dram("h2T", [8, 128, S], BF16)
        self.uT_d = self.dram("uT", [8, 128, NE], BF16)
        self.vb_d = self.dram("vb", [NE, D], BF16)

        with ExitStack() as st:
            self.consts(st)
            for l in range(L):
                self.layer(l)
                if self.stop_after is not None and self.stop_after[0] == l and self.stop_after[1] != "D":
                    break
            self.sc.emit(nc)
        return nc

    def consts(self, st):
        S, NT = self.S, self.NT
        sb = lambda n, s, d: self.sb(st, n, s, d)
        self.ident = sb("ident", [128, 128], BF16)
        self.identf = sb("identf", [128, 128], F32)
        self.cneg = sb("cneg", [128, 128], F32)
        self.cpos = sb("cpos", [128, 128], F32)
        self.onesf = sb("onesf", [1, 128], F32)
        self.cosT = sb("cosT", [128, NT, 8], F32)
        self.sinT = sb("sinT", [128, NT, 8], F32)
        self.cosq = sb("cosq", [128, NT, 8], F32)
        self.sinq = sb("sinq", [128, NT, 8], F32)
        self.wres = sb("wres", [128, NT, 8], F32)
        self.invc0 = sb("invc0", [128, 4, 128], F32)
        self.invcg = sb("invcg", [128, 4, 128], F32)
        self.modb = sb("modb", [128, 6 * D], F32)
        self.A1 = sb("A1", [128, D], F32)
        self.A2 = sb("A2", [128, D], F32)
        self.dummy = sb("bar_dummy", [128, 1], F32)
        with ExitStack() as t:
            ii = self.sb(t, "ii", [128, 128], I32)
            fi = self.sb(t, "fi", [128, 128], F32)
            posi = self.sb(t, "posi", [128, NT], I32)
            posf = self.sb(t, "posf", [128, NT], F32)
            ang = self.sb(t, "ang", [128, NT, 8], F32)
            t1 = self.sb(t, "rt1", [128, NT, 8], F32)
            ki = self.sb(t, "rki", [128, NT, 8], I32)
            kf = self.sb(t, "rkf", [128, NT, 8], F32)
            t2 = self.sb(t, "rt2", [128, NT, 8], F32)
            self.op("pool", lambda e: e.iota(ii[:], pattern=[[1, 128]], base=0, channel_multiplier=-1), w=["ii"])
            self.copy("dve", fi[:], ii[:], ["ii"], ["fi"])
            self.ts("dve", self.identf[:], fi[:], 0.0, ALU.is_equal, ["fi"], ["identf"])
            self.copy("dve", self.ident[:], self.identf[:], ["identf"], ["ident"])
            self.ts("dve", self.cneg[:], fi[:], 0.0, ALU.is_gt, ["fi"], ["cneg"], s2=NEG, op1=ALU.mult)
            self.ts("dve", self.cpos[:], fi[:], 0.0, ALU.is_gt, ["fi"], ["cpos"], s2=-NEG, op1=ALU.mult)
            self.op("dve", lambda e: e.memset(self.onesf[:], 1.0), w=["onesf"])
            i2 = self.sb(t, "i2", [128, 128], I32)
            f2 = self.sb(t, "f2", [128, 128], F32)
            f3 = self.sb(t, "f3", [128, 128], F32)
            self.op("pool", lambda e: e.iota(i2[:], pattern=[[1, 128]], base=1, channel_multiplier=0), w=["i2"])
            self.copy("dve", f2[:], i2[:], ["i2"], ["f2"])
            for g in range(4):
                wv = float(2 ** (g + 1))
                self.ts("dve", f3[:], f2[:], wv, ALU.min, ["f2"], ["f3"])
                self.op("dve", lambda e, g=g: e.reciprocal(out=self.invc0[:, g, :], in_=f3[:]), r=["f3"], w=["invc0"])
                self.op("dve", lambda e, g=g, wv=wv: e.memset(self.invcg[:, g, :], 1.0 / wv), w=["invcg"])
            self.dma(posi[:], self.pos_d[:, :], w=["posi"])
            self.copy("dve", posf[:], posi[:], ["posi"], ["posf"])
            inv = (500000.0 ** (-np.arange(0, 16, 2, dtype=np.float32) / np.float32(16))).astype(np.float32)
            for j in range(8):
                self.ts("dve", ang[:, :, j:j + 1], posf[:].rearrange("p (n o) -> p n o", o=1), float(inv[j]), ALU.mult, ["posf"], ["ang"])
            C1 = 6.28125
            C2 = float(2 * np.pi - 6.28125)
            PI = 3.1415925
            for which, (tab, tabq) in enumerate([(self.sinT, self.sinq), (self.cosT, self.cosq)]):
                src = ang
                if which == 1:
                    self.ts("dve", t2[:], ang[:], float(np.pi / 2), ALU.add, ["ang"], ["t2"])
                    src = t2
                rr = ["ang", "t2"]
                self.ts("dve", t1[:], src[:], float(1 / (2 * np.pi)), ALU.mult, rr, ["t1"])
                self.copy("dve", ki[:], t1[:], ["t1"], ["ki"])
                self.copy("dve", kf[:], ki[:], ["ki"], ["kf"])
                self.stt(t1[:], kf[:], -C1, src[:], ALU.mult, ALU.add, ["kf"] + rr, ["t1"])
                self.stt(t1[:], kf[:], -C2, t1[:], ALU.mult, ALU.add, ["kf", "t1"], ["t1"])
                self.ts("dve", kf[:], t1[:], PI, ALU.is_gt, ["t1"], ["kf"], s2=-2 * np.pi, op1=ALU.mult)
                self.tt("dve", t1[:], t1[:], kf[:], ALU.add, ["t1", "kf"], ["t1"])
                self.ts("dve", kf[:], t1[:], -PI, ALU.is_lt, ["t1"], ["kf"], s2=2 * np.pi, op1=ALU.mult)
                self.tt("dve", t1[:], t1[:], kf[:], ALU.add, ["t1", "kf"], ["t1"])
                self.ts("dve", t1[:], t1[:], PI, ALU.min, ["t1"], ["t1"], s2=-PI, op1=ALU.max)
                self.act(tab[:], t1[:], AF.Sin, ["t1"], ["rope"])
                self.ts("dve", tabq[:], tab[:], 0.125, ALU.mult, ["rope"], ["rope"])
            self.barrier()

    def layer(self, l):
        if self.stop_after == (l, "consts"):
            return
        self.mod_vectors(l)
        if self.stop_after == (l, "mod"):
            return
        self.phase_a(l)
        if self.stop_after == (l, "A"):
            return
        self.phase_b(l)
        if self.stop_after == (l, "B"):
            return
        self.phase_c(l)
        if self.stop_after == (l, "C"):
            return
        self.phase_d(l)

    def mod_vectors(self, l):
        with ExitStack() as st:
            ct = self.sb(st, "ct", [128, 8], F32)
            scs = self.sb(st, "scs", [128, 8], F32)
            scb = self.sb(st, "scb", [128, 8, 128], F32)
            wst = [self.sb(st, "wada%d" % i, [128, 8, 512], F32) for i in range(2)]
            brow = self.sb(st, "brow", [1, 6 * D], F32)
            nmb = self.sb(st, "nmb", [128, D], F32)
            nfb = self.sb(st, "nfb", [128, D], F32)
            pm = [self.ps(st, "pmod%d" % i, [128, 512], F32) for i in range(2)]
            self.dma(ct[:], self.c_d[:, :], w=["ct"])
            self.act(scs[:], ct[:], AF.Silu, ["ct"], ["scs"])
            self.copy("dve", scb[:], scs[:].rearrange("p (k o) -> p k o", o=1).to_broadcast([128, 8, 128]), ["scs"], ["scb"])
            self.dma(brow[:], self.b_ada[l:l + 1, :], w=["brow"])
            self.dma(nmb[:], self.norm_mix[l:l + 1, :].to_broadcast([128, D]), w=["nmb"])
            self.dma(nfb[:], self.norm_ffn[l:l + 1, :].to_broadcast([128, D]), w=["nfb"])
            wv = self.w_ada[l].rearrange("(k p) n -> p k n", p=128)
            for j in range(12):
                b = j % 2
                self.dma(wst[b][:], wv[:, :, j * 512:(j + 1) * 512], w=["wada%d" % b])
                specs = [(pm[b][:], scb[:, k, :], wst[b][:, k, :], k == 0, False) for k in range(8)]
                specs.append((pm[b][:], self.onesf[:], brow[:, j * 512:(j + 1) * 512], False, True))
                self.mms(specs, ["scb", "wada%d" % b, "brow", "onesf"], ["P:pmod%d" % b])
                self.copy("act", self.modb[:, j * 512:(j + 1) * 512], pm[b][:], ["P:pmod%d" % b], ["modb"])
            self.stt(self.A1[:], self.modb[:, D:2 * D], 1.0, nmb[:], ALU.add, ALU.mult, ["modb", "nmb"], ["A1"])
            self.stt(self.A2[:], self.modb[:, 4 * D:5 * D], 1.0, nfb[:], ALU.add, ALU.mult, ["modb", "nfb"], ["A2"])
            self.barrier()

    def load_weight_bf(self, st, name, src_view, shape, nk, stage):
        wt = self.sb(st, name, shape, BF16) if not isinstance(st, tuple) else st[0]
        for k in range(nk):
            b = k % 2
            self.dma(stage[b][:, 0:shape[2]], src_view[:, k, :], w=["stage%d" % b])
            eng = "act" if b == 0 else "pool"
            self.copy(eng, wt[:, k, :], stage[b][:, 0:shape[2]], ["stage%d" % b], [name])
        return wt

    def rms_h(self, xt, A, sh, hb, tmp, junk, ss, rstd, pfx, rx):
        self.act(junk, xt, AF.Square, rx, [pfx + "junk", pfx + "ss"], accum_out=ss)
        self.act(ss, ss, AF.Sqrt, [pfx + "ss"], [pfx + "ss"], bias=1e-6, scale=1.0 / D)
        self.op("dve", lambda e: e.reciprocal(out=rstd, in_=ss), [pfx + "ss"], [pfx + "rstd"])
        self.stt(tmp, xt, rstd, A, ALU.mult, ALU.mult, rx + [pfx + "rstd", "A1", "A2"], [pfx + "tmp"])
        self.tt("dve", hb, tmp, sh, ALU.add, [pfx + "tmp", "modb"], [pfx + "hb"])

    def phase_a(self, l):
        S, NT = self.S, self.NT
        xsrc = self.x_d if l == 0 else self.xres_d
        xkey = "x_in" if l == 0 else "xres"
        with ExitStack() as st:
            stage = [self.sb(st, "stage%d" % i, [128, DIN], F32) for i in range(2)]
            win = self.load_weight_bf(st, "win", self.w_in[l].rearrange("(k p) n -> p k n", p=128), [128, 8, DIN], 8, stage)
            wpf = self.sb(st, "wpf", [128, 4, 128], F32)
            psb = self.sb(st, "psb", [128, 4, 128], F32)
            wpool = self.sb(st, "wpool", [128, 4, 128], BF16)
            self.dma(wpf[:], self.w_pool[l].rearrange("g c d -> c g d"), w=["wpf"])
            self.dma(psb[:], self.pool_scale[l:l + 1, :].rearrange("o (g d) -> o g d", g=4).to_broadcast([128, 4, 128]), w=["psb"])
            self.tt("dve", wpool[:], wpf[:], psb[:], ALU.mult, ["wpf", "psb"], ["wpool"])

            xt = [self.sb(st, "xt%d" % i, [128, D], F32) for i in range(2)]
            junk = self.sb(st, "junk", [128, D], BF16)
            tmp = self.sb(st, "tmpA", [128, D], F32)
            hb = self.sb(st, "hb", [128, D], BF16)
            hT = [self.sb(st, "hT%d" % i, [128, 8, 128], BF16) for i in range(2)]
            ss = self.sb(st, "ssA", [128, 1], F32)
            rstd = self.sb(st, "rstdA", [128, 1], F32)
            xpw = self.sb(st, "xpw", [128, 4, 143], F32)
            ya = self.sb(st, "ya", [128, 4, 143], F32)
            yb = self.sb(st, "yb", [128, 4, 143], F32)
            yc = self.sb(st, "yc", [128, 4, 143], F32)
            yd = self.sb(st, "yd", [128, 4, 143], F32)
            ptmp = self.sb(st, "ptmp", [128, 4, 128], F32)
            pooledT = self.sb(st, "pooledT", [128, 4, 128], BF16)
            mixp = self.sb(st, "mixp", [128, 4, 128], BF16)
            qb = [self.sb(st, "qb%d" % i, [128, 512], BF16) for i in range(3)]
            kib = self.sb(st, "kib", [128, 128], BF16)
            vaug = self.sb(st, "vaugs", [128, 8, 65], BF16)
            r1 = self.sb(st, "r1", [128, 8, 8], F32)
            r2 = self.sb(st, "r2", [128, 8, 8], F32)
            qTs = [self.sb(st, "qTs%d" % i, [128, 4, 128], BF16) for i in range(3)]
            kiTs = self.sb(st, "kiTs", [128, 128], BF16)

            pT = self.ps(st, "pT", [128, 8, 128], BF16)
            pxp = self.ps(st, "pxp", [128, 4, 128], F32)
            pmx = self.ps(st, "pmx", [128, 4, 128], F32)
            pj = [self.ps(st, "pj%d" % i, [128, 512], F32) for i in range(3)]
            ptr = [self.ps(st, "ptr%d" % i, [128, 8, 128], BF16) for i in range(2)]

            self.op("pool", lambda e: e.memset(xpw[:], 0.0), w=["xpw"])
            self.op("pool", lambda e: e.memset(vaug[:], 1.0), w=["vaugs"])

            def load_x(i):
                b = i % 2
                self.dma(xt[b][:], xsrc[i * 128:(i + 1) * 128, :], r=[(xkey, i)], w=["xt%d" % b])

            import os as _os
            ASTG = int(_os.environ.get("ASTG", "9"))
            load_x(0)
            for i in range(NT if ASTG > 0 else 0):
                b = i % 2
                if i + 1 < NT:
                    load_x(i + 1)
                sl = slice(i * 128, (i + 1) * 128)
                self.rms_h(xt[b][:], self.A1[:], self.modb[:, 0:D], hb[:], tmp[:], junk[:], ss[:], rstd[:], "A", ["xt%d" % b])
                self.transposes([(pT[:, k, :], hb[:, k * 128:(k + 1) * 128]) for k in range(8)], self.ident[:], ["Ahb", "ident"], ["P:pT"])
                self.copy("act", hT[b][:], pT[:], ["P:pT"], ["hT%d" % b])
                hk = "hT%d" % b
                if ASTG < 2:
                    continue
                specs = []
                for g in range(4):
                    for k in range(8):
                        specs.append((pxp[:, g, :], win[:, k, g * 128:(g + 1) * 128], hT[b][:, k, :], k == 0, k == 7))
                self.mms(specs, [hk, "win"], ["P:pxp"])
                self.copy("act", xpw[:, :, 15:143], pxp[:], ["P:pxp"], ["xpw"])
                self.tt("pool", ya[:, :, 1:143], xpw[:, :, 1:143], xpw[:, :, 0:142], ALU.add, ["xpw"], ["ya"])
                self.tt("pool", yb[:, 1:4, 3:143], ya[:, 1:4, 3:143], ya[:, 1:4, 1:141], ALU.add, ["ya"], ["yb"])
                self.tt("pool", yc[:, 2:4, 7:143], yb[:, 2:4, 7:143], yb[:, 2:4, 3:139], ALU.add, ["yb"], ["yc"])
                self.tt("pool", yd[:, 3:4, 15:143], yc[:, 3:4, 15:143], yc[:, 3:4, 7:135], ALU.add, ["yc"], ["yd"])
                invc = self.invc0 if i == 0 else self.invcg
                srcs = [ya, yb, yc, yd]
                for g in range(4):
                    self.tt("pool", ptmp[:, g, :], srcs[g][:, g, 15:143], invc[:, g, :], ALU.mult,
                            ["ya", "yb", "yc", "yd", "invc0", "invcg"], ["ptmp"])
                self.tt("pool", pooledT[:], ptmp[:], xpw[:, :, 15:143], ALU.subtract, ["ptmp", "xpw"], ["pooledT"])
                self.copy("pool", xpw[:, :, 0:15], xpw[:, :, 128:143], ["xpw", "pooledT"], ["xpw"])
                self.mms([(pmx[:, g, :], wpool[:, g, :], pooledT[:, g, :], True, True) for g in range(4)],
                         ["wpool", "pooledT"], ["P:pmx"])
                self.copy("act", mixp[:], pmx[:], ["P:pmx"], ["mixp"])
                self.dma(self.mixT_d[0:4, :, sl].rearrange("k p t -> p k t"), mixp[:], r=["mixp"], w=[("mixT", i, 0)])
                if ASTG < 3:
                    continue
                for j in [int(c_) for c_ in _os.environ.get("AJ", "12345")]:
                    pb = pj[j % 3]
                    pk = "P:pj%d" % (j % 3)
                    n0 = j * 512
                    nn = min(512, DIN - n0)
                    self.mms([(pb[:, 0:nn], hT[b][:, k, :], win[:, k, n0:n0 + nn], k == 0, k == 7) for k in range(8)],
                             [hk, "win"], [pk])
                    if j in (1, 2, 4):
                        qi_ = {1: 0, 2: 1, 4: 2}[j]
                        dst = qb[qi_]
                        dk = "qb%d" % qi_
                        scaled = j in (1, 4)
                        ct_ = self.cosq if scaled else self.cosT
                        st_ = self.sinq if scaled else self.sinT
                        if int(_os.environ.get("QS", "9")) < 0:
                            continue
                        if scaled:
                            self.op("act", lambda e, dst=dst, pb=pb: e.activation(out=dst[:], in_=pb[:], func=AF.Identity, scale=0.125), [pk], [dk])
                        else:
                            self.copy("act", dst[:], pb[:], [pk], [dk])
                        QS = int(_os.environ.get("QS", "9"))
                        if QS < 1:
                            continue
                        pv = pb[:].rearrange("p (h d) -> p h d", h=8)
                        dv = dst[:].rearrange("p (h d) -> p h d", h=8)
                        cb = ct_[:, i:i + 1, :].to_broadcast([128, 8, 8])
                        sbb = st_[:, i:i + 1, :].to_broadcast([128, 8, 8])
                        self.tt("dve", r1[:], pv[:, :, 0:8], cb, ALU.mult, [pk, "rope"], ["r1"])
                        self.tt("dve", r2[:], pv[:, :, 8:16], sbb, ALU.mult, [pk, "rope"], ["r2"])
                        self.tt("dve", dv[:, :, 0:8], r1[:], r2[:], ALU.subtract, ["r1", "r2"], [dk])
                        self.tt("dve", r1[:], pv[:, :, 0:8], sbb, ALU.mult, [pk, "rope", dk], ["r1"])
                        self.tt("dve", r2[:], pv[:, :, 8:16], cb, ALU.mult, [pk, "rope", dk], ["r2"])
                        self.tt("dve", dv[:, :, 8:16], r1[:], r2[:], ALU.add, ["r1", "r2"], [dk])
                        if QS < 2:
                            continue
                        pt = ptr[qi_ % 2]
                        ptk = "P:ptr%d" % (qi_ % 2)
                        self.transposes([(pt[:, p_, :], dst[:, p_ * 128:(p_ + 1) * 128]) for p_ in range(4)], self.ident[:], [dk, "ident"], [ptk])
                        self.copy("act", qTs[qi_][:], pt[:, 0:4, :], [ptk], ["qTs%d" % qi_])
                        if QS < 3:
                            continue
                        dd = {0: self.qT_d, 1: self.kT_d, 2: self.qiT_d}[qi_]
                        nm = {0: "qT", 1: "kT", 2: "qiT"}[qi_]
                        self.dma(dd[:, :, sl].rearrange("k p t -> p k t"), qTs[qi_][:], r=["qTs%d" % qi_], w=[(nm, i)])
                    elif j == 3:
                        self.copy("act", vaug[:, :, 0:64], pb[:].rearrange("p (h d) -> p h d", h=8), [pk], ["vaugs"])
                        self.dma(self.vaug_d[sl, :], vaug[:].rearrange("p h d -> p (h d)"), r=["vaugs"], w=[("vaug", i)])
                    else:
                        self.copy("act", kib[:, 0:64], pb[:, 0:64], [pk], ["kib"])
                        cb = self.cosT[:, i, :]
                        sbb = self.sinT[:, i, :]
                        r1v = r1[:, 0, :]
                        r2v = r2[:, 0, :]
                        self.tt("dve", r1v, pb[:, 0:8], cb, ALU.mult, [pk, "rope"], ["r1"])
                        self.tt("dve", r2v, pb[:, 8:16], sbb, ALU.mult, [pk, "rope"], ["r2"])
                        self.tt("dve", kib[:, 0:8], r1v, r2v, ALU.subtract, ["r1", "r2"], ["kib"])
                        self.tt("dve", r1v, pb[:, 0:8], sbb, ALU.mult, [pk, "rope", "kib"], ["r1"])
                        self.tt("dve", r2v, pb[:, 8:16], cb, ALU.mult, [pk, "rope", "kib"], ["r2"])
                        self.tt("dve", kib[:, 8:16], r1v, r2v, ALU.add, ["r1", "r2"], ["kib"])
                        self.copy("dve", kib[:, 64:128], kib[:, 0:64], ["kib"], ["kib"])
                        self.ts("dve", self.wres[:, i, :], pb[:, 64:72], float(8 ** -0.5), ALU.mult, [pk], ["wres"])
                        pt = ptr[1]
                        self.transposes([(pt[:, 0, :], kib[:])], self.ident[:], ["kib", "ident"], ["P:ptr1"])
                        self.copy("act", kiTs[:], pt[:, 0, :], ["P:ptr1"], ["kiTs"])
                        self.dma(self.kiT_d[:, sl], kiTs[:], r=["kiTs"], w=[("kiT", i)])
            self.barrier()

    def phase_b(self, l):
        S, NT = self.S, self.NT
        TOPK = self.topk
        NBIS = 22
        with ExitStack() as st:
            sb = lambda n, sh, d: self.sb(st, n, sh, d)
            kiT = sb("kiTr", [128, S], BF16)
            self.dma(kiT[:], self.kiT_d[:, :], r=[("kiT", i) for i in range(NT)], w=["kiTr"])
            scores = sb("scores", [128, S], F32)
            Bm = sb("Bm", [128, S], BF16)
            junk = sb("junkB", [128, S], BF16)
            qiT_t = [sb("qiTt%d" % i, [128, 4, 128], BF16) for i in range(2)]
            qT_t = [sb("qTt%d" % i, [128, 4, 128], BF16) for i in range(2)]
            kTg = [sb("kTg%d" % i, [128, 4, 512], BF16) for i in range(2)]
            vg = [sb("vg%d" % i, [128, 4, 520], BF16) for i in range(2)]
            ET = [sb("ET%d" % i, [128, 512], BF16) for i in range(2)]
            lo = sb("lo", [128, 1], F32)
            half = sb("half", [128, 1], F32)
            mid = sb("mid", [128, 1], F32)
            cnt = sb("cnt", [128, 1], F32)
            tq = sb("tq", [128, 1], F32)
            mn1 = sb("mn1", [128, 1], F32)
            mn2 = sb("mn2", [128, 1], F32)
            hi = sb("hi", [128, 1], F32)
            tmpd = sb("tmpd", [128, 128], F32)
            Oacc = sb("Oacc", [128, 520], F32)
            rz = sb("rz", [128, 8], F32)
            attb = sb("attb", [128, 512], BF16)
            aTs = sb("aTs", [128, 4, 128], BF16)
            psb_ = [self.ps(st, "psB%d" % i, [128, 512], F32) for i in range(2)]
            rp = self.ps(st, "rpB", [128, 512], F32)
            pl = [self.ps(st, "plB%d" % i, [128, 512], F32) for i in range(2)]
            po = [self.ps(st, "poB%d" % i, [128, 512], F32) for i in range(2)]
            ptrb = self.ps(st, "ptrB", [128, 8, 128], BF16)
            nps = 0
            npl = 0
            for i in range(NT):
                b = i % 2
                sl = slice(i * 128, (i + 1) * 128)
                Lk = (i + 1) * 128
                nkb = i + 1
                self.dma(qiT_t[b][:], self.qiT_d[:, :, sl].rearrange("k p t -> p k t"), r=[("qiT", i)], w=["qiTt%d" % b])
                self.dma(qT_t[b][:], self.qT_d[:, :, sl].rearrange("k p t -> p k t"), r=[("qT", i)], w=["qTt%d" % b])
                for c in range((Lk + 511) // 512):
                    ncol = min(512, Lk - c * 512)
                    cs = slice(c * 512, c * 512 + ncol)
                    for h in range(8):
                        pr, base = h // 2, (h % 2) * 64
                        pb = psb_[nps % 2]
                        pk = "P:psB%d" % (nps % 2)
                        nps += 1
                        self.mms([(pb[:, 0:ncol], qiT_t[b][base:base + 64, pr, :], kiT[base:base + 64, cs], True, True)],
                                 ["qiTt%d" % b, "kiTr"], [pk])
                        self.act(rp[:, 0:ncol], pb[:, 0:ncol], AF.Relu, [pk], ["P:rpB"])
                        wv = self.wres[:, i, h:h + 1]
                        if h == 0:
                            self.ts("dve", scores[:, cs], rp[:, 0:ncol], wv, ALU.mult, ["P:rpB", "wres"], ["scores"])
                        else:
                            self.stt(scores[:, cs], rp[:, 0:ncol], wv, scores[:, cs], ALU.mult, ALU.add, ["P:rpB", "wres", "scores"], ["scores"])
                dg = slice(i * 128, Lk)
                self.tt("dve", tmpd[:], scores[:, dg], self.cpos[:], ALU.add, ["scores", "cpos"], ["tmpd"])
                self.op("dve", lambda e: e.tensor_reduce(out=mn2[:], in_=tmpd[:], axis=AX.X, op=ALU.min), ["tmpd"], ["mn2"])
                if i > 0:
                    self.op("dve", lambda e, i=i: e.tensor_reduce(out=mn1[:], in_=scores[:, 0:i * 128], axis=AX.X, op=ALU.min), ["scores"], ["mn1"])
                    self.tt("dve", lo[:], mn1[:], mn2[:], ALU.min, ["mn1", "mn2"], ["lo"])
                else:
                    self.copy("dve", lo[:], mn2[:], ["mn2"], ["lo"])
                self.tt("dve", scores[:, dg], scores[:, dg], self.cneg[:], ALU.add, ["scores", "cneg", "tmpd"], ["scores"])
                if Lk > TOPK:
                    self.op("dve", lambda e, Lk=Lk: e.tensor_reduce(out=hi[:], in_=scores[:, 0:Lk], axis=AX.X, op=ALU.max), ["scores"], ["hi"])
                    self.tt("dve", half[:], hi[:], lo[:], ALU.subtract, ["hi", "lo"], ["half"])
                    self.ts("dve", half[:], half[:], 0.5, ALU.mult, ["half"], ["half"])
                    for it in range(NBIS):
                        self.tt("dve", mid[:], lo[:], half[:], ALU.add, ["lo", "half"], ["mid"])
                        self.ts("dve", junk[:, 0:Lk], scores[:, 0:Lk], mid[:], ALU.is_ge, ["scores", "mid"], ["junkB", "cnt"],
                                op1=ALU.add, accum_out=cnt[:])
                        self.ts("dve", tq[:], cnt[:], float(TOPK) - 0.5, ALU.is_ge, ["cnt", "half"], ["tq"], s2=half[:], op1=ALU.mult)
                        self.tt("dve", lo[:], lo[:], tq[:], ALU.add, ["lo", "tq"], ["lo"])
                        self.ts("dve", half[:], half[:], 0.5, ALU.mult, ["half", "tq"], ["half"])
                self.ts("dve", Bm[:, 0:Lk], scores[:, 0:Lk], lo[:], ALU.is_lt, ["scores", "lo"], ["Bm"], s2=-30000.0, op1=ALU.mult)
                ngr = (nkb + 3) // 4
                for kg in range(ngr):
                    nb = min(4, nkb - kg * 4)
                    gb = kg % 2
                    k0 = kg * 512
                    self.dma(kTg[gb][:, :, 0:nb * 128], self.kT_d[:, :, k0:k0 + nb * 128].rearrange("k p t -> p k t"),
                             r=[("kT", kg * 4 + j) for j in range(nb)], w=["kTg%d" % gb])
                    self.dma(vg[gb][:, 0:nb, :], self.vaug_d[k0:k0 + nb * 128, :].rearrange("(j p) n -> p j n", p=128),
                             r=[("vaug", kg * 4 + j) for j in range(nb)], w=["vg%d" % gb])
                    for h in range(8):
                        pr, base = h // 2, (h % 2) * 64
                        plb = pl[npl % 2]
                        plk = "P:plB%d" % (npl % 2)
                        etb = ET[npl % 2]
                        etk = "ET%d" % (npl % 2)
                        npl += 1
                        specs = []
                        for jb in range(nb):
                            js = slice(jb * 128, (jb + 1) * 128)
                            specs.append((plb[:, js], kTg[gb][base:base + 64, pr, js], qT_t[b][base:base + 64, pr, :], True, False))
                            specs.append((plb[:, js], Bm[:, k0 + jb * 128:k0 + (jb + 1) * 128], self.ident[:], False, True))
                        self.mms(specs, ["kTg%d" % gb, "qTt%d" % b, "Bm", "ident"], [plk])
                        self.act(etb[:, 0:nb * 128], plb[:, 0:nb * 128], AF.Exp, [plk], [etk])
                        pob = po[h // 4]
                        pok = "P:poB%d" % (h // 4)
                        hh = h % 4
                        specs = [(pob[:, hh * 65:(hh + 1) * 65], etb[:, jb * 128:(jb + 1) * 128], vg[gb][:, jb, h * 65:(h + 1) * 65],
                                  jb == 0, jb == nb - 1) for jb in range(nb)]
                        self.mms(specs, [etk, "vg%d" % gb], [pok])
                    for x_ in range(2):
                        if kg == 0:
                            self.copy("dve", Oacc[:, x_ * 260:(x_ + 1) * 260], po[x_][:, 0:260], ["P:poB%d" % x_], ["Oacc"])
                        else:
                            self.tt("dve", Oacc[:, x_ * 260:(x_ + 1) * 260], po[x_][:, 0:260], Oacc[:, x_ * 260:(x_ + 1) * 260], ALU.add,
                                    ["P:poB%d" % x_, "Oacc"], ["Oacc"])
                Ov = Oacc[:].rearrange("p (h d) -> p h d", h=8)
                self.op("dve", lambda e, Ov=Ov: e.reciprocal(out=rz[:].rearrange("p (h o) -> p h o", o=1), in_=Ov[:, :, 64:65]), ["Oacc"], ["rz"])
                self.tt("dve", attb[:].rearrange("p (h d) -> p h d", h=8), Ov[:, :, 0:64],
                        rz[:].rearrange("p (h o) -> p h o", o=1).to_broadcast([128, 8, 64]), ALU.mult, ["Oacc", "rz"], ["attb"])
                self.transposes([(ptrb[:, p_, :], attb[:, p_ * 128:(p_ + 1) * 128]) for p_ in range(4)], self.ident[:], ["attb", "ident"], ["P:ptrB"])
                self.copy("act", aTs[:], ptrb[:, 0:4, :], ["P:ptrB"], ["aTs"])
                self.dma(self.mixT_d[4:8, :, sl].rearrange("k p t -> p k t"), aTs[:], r=["aTs"], w=[("mixT", i, 1)])
            self.barrier()

    def phase_c(self, l):
        S, NT = self.S, self.NT
        xsrc = self.x_d if l == 0 else self.xres_d
        xkey = "x_in" if l == 0 else "xres"
        with ExitStack() as st:
            sb = lambda n, sh, d: self.sb(st, n, sh, d)
            stage = [sb("stage%d" % i, [128, DIN], F32) for i in range(2)]
            wout = self.load_weight_bf(st, "wout", self.w_out[l].rearrange("(k p) n -> p k n", p=128), [128, 8, D], 8, stage)
            xt = [sb("xtC%d" % i, [128, D], F32) for i in range(2)]
            mT = [sb("mTC%d" % i, [128, 8, 128], BF16) for i in range(2)]
            xn = sb("xnC", [128, D], F32)
            tmp = sb("tmpC", [128, D], F32)
            junk = sb("junkC", [128, D], BF16)
            hb = sb("hbC", [128, D], BF16)
            hT = sb("hTC", [128, 8, 128], BF16)
            ss = sb("ssC", [128, 1], F32)
            rstd = sb("rstdC", [128, 1], F32)
            py = [self.ps(st, "pyC%d" % i, [128, 512], F32) for i in range(2)]
            pT = self.ps(st, "pTC", [128, 8, 128], BF16)

            def loads(i):
                b = i % 2
                sl = slice(i * 128, (i + 1) * 128)
                self.dma(mT[b][:], self.mixT_d[:, :, sl].rearrange("k p t -> p k t"), r=[("mixT", i, 0), ("mixT", i, 1)], w=["mTC%d" % b])
                self.dma(xt[b][:], xsrc[sl, :], r=[(xkey, i)], w=["xtC%d" % b])

            loads(0)
            for i in range(NT):
                b = i % 2
                sl = slice(i * 128, (i + 1) * 128)
                if i + 1 < NT:
                    loads(i + 1)
                for hf in range(2):
                    self.mms([(py[hf][:], mT[b][:, k, :], wout[:, k, hf * 512:(hf + 1) * 512], k == 0, k == 7) for k in range(8)],
                             ["mTC%d" % b, "wout"], ["P:pyC%d" % hf])
                    hs = slice(hf * 512, (hf + 1) * 512)
                    self.tt("dve", tmp[:, hs], py[hf][:], self.modb[:, 2 * D + hf * 512:2 * D + (hf + 1) * 512], ALU.mult,
                            ["P:pyC%d" % hf, "modb"], ["tmpC"])
                self.tt("dve", xn[:], tmp[:], xt[b][:], ALU.add, ["tmpC", "xtC%d" % b], ["xnC"])
                self.dma(self.xres_d[sl, :], xn[:], r=["xnC"], w=[("xres", i)])
                self.rms_h(xn[:], self.A2[:], self.modb[:, 3 * D:4 * D], hb[:], tmp[:], junk[:], ss[:], rstd[:], "C", ["xnC"])
                self.transposes([(pT[:, k, :], hb[:, k * 128:(k + 1) * 128]) for k in range(8)], self.ident[:], ["Chb", "ident"], ["P:pTC"])
                self.copy("act", hT[:], pT[:], ["P:pTC"], ["hTC"])
                self.dma(self.h2T_d[:, :, sl].rearrange("k p t -> p k t"), hT[:], r=["hTC"], w=[("h2T", i)])
            self.barrier()

    def phase_d(self, l):
        S, NT = self.S, self.NT
        last = (l == self.L - 1)
        with ExitStack() as st:
            sb = lambda n, sh, d: self.sb(st, n, sh, d)
            ust = [sb("ust%d" % i, [128, D], F32) for i in range(2)]
            ubf = [sb("ubf%d" % i, [128, D], BF16) for i in range(2)]
            uTs = [sb("uTs%d" % i, [128, 8, 512], BF16) for i in range(2)]
            vst = [sb("vst%d" % i, [128, 4, D], F32) for i in range(2)]
            vbs = [sb("vbs%d" % i, [128, 4, D], BF16) for i in range(2)]
            pT = [self.ps(st, "pTU%d" % i, [128, 8, 128], BF16) for i in range(2)]
            n = 0
            for g4 in range(NE // 512):
                ub = g4 % 2
                for q in range(4):
                    b = n % 2
                    n += 1
                    e0 = g4 * 512 + q * 128
                    self.dma(ust[b][:], self.expert_u[l, e0:e0 + 128, :], w=["ust%d" % b])
                    self.copy("act" if b == 0 else "pool", ubf[b][:], ust[b][:], ["ust%d" % b], ["ubf%d" % b])
                    self.transposes([(pT[b][:, k, :], ubf[b][:, k * 128:(k + 1) * 128]) for k in range(8)], self.ident[:],
                                    ["ubf%d" % b, "ident"], ["P:pTU%d" % b])
                    self.copy("dve", uTs[ub][:, :, q * 128:(q + 1) * 128], pT[b][:], ["P:pTU%d" % b], ["uTs%d" % ub])
                self.dma(self.uT_d[:, :, g4 * 512:(g4 + 1) * 512].rearrange("k d e -> d k e"), uTs[ub][:], r=["uTs%d" % ub], w=[("uT", g4)])
                self.dma(vst[ub][:], self.expert_v[l, g4 * 512:(g4 + 1) * 512, :].rearrange("(a p) n -> p a n", p=128), w=["vst%d" % ub])
                self.copy("act" if ub == 0 else "pool", vbs[ub][:], vst[ub][:], ["vst%d" % ub], ["vbs%d" % ub])
                self.dma(self.vb_d[g4 * 512:(g4 + 1) * 512, :].rearrange("(a p) n -> p a n", p=128), vbs[ub][:], r=["vbs%d" % ub], w=[("vb", g4)])
            self.barrier()
        TS = 256
        NTT = TS // 128
        with ExitStack() as st:
            sb = lambda n, sh, d: self.sb(st, n, sh, d)
            skT = sb("skT", [128, 16, 128], BF16)
            fnb = sb("fnb", [128, D], F32)
            wq = sb("wq", [128, 8, 2048], BF16)
            with ExitStack() as st2:
                stage = [self.sb(st2, "stage%d" % i, [128, DIN], F32) for i in range(2)]
                self.load_weight_bf((wq,), "wq", self.w_query[l].rearrange("(k p) n -> p k n", p=128), [128, 8, 2048], 8, stage)
                skf = self.sb(st2, "skf", [128, 16, 128], F32)
                skb = self.sb(st2, "skb", [128, 16, 128], BF16)
                pTs = [self.ps(st2, "pTS%d" % i, [128, 8, 128], BF16) for i in range(2)]
                self.dma(skf[:], self.sub_keys[l].rearrange("h p n d -> n (h p) d"), w=["skf"])
                self.copy("dve", skb[:], skf[:], ["skf"], ["skb"])
                for g in range(2):
                    self.transposes([(pTs[g][:, k, :], skb[:, g * 8 + k, :]) for k in range(8)], self.ident[:], ["skb", "ident"], ["P:pTS%d" % g])
                    self.copy("act", skT[:, g * 8:(g + 1) * 8, :], pTs[g][:], ["P:pTS%d" % g], ["skT"])
                self.barrier()
            self.dma(fnb[:], self.final_norm[0:1, :].to_broadcast([128, D]), w=["fnb"])
            h2s = sb("h2s", [128, 8, TS], BF16)
            qTs = sb("qTsD", [128, 16, TS], BF16)
            s_sb = [sb("s_sb%d" % i, [128, 16, 128], F32) for i in range(NTT)]
            t16 = sb("t16", [128, 16, 16], F32)
            mr = sb("mrD", [128, 128], F32)
            cand = sb("cand", [128, 256], F32)
            cand2 = sb("cand2", [128, 256], F32)
            c16 = sb("c16", [128, 8, 16], F32)
            e16 = sb("e16", [128, 8, 16], F32)
            Zs = sb("Zs", [128, 8], F32)
            thr_t = [sb("thr_t%d" % i, [128, 8], F32) for i in range(NTT)]
            nb_t = [sb("nb_t%d" % i, [128, 8], F32) for i in range(NTT)]
            uTc = [sb("uTc%d" % i, [128, 8, 512], BF16) for i in range(2)]
            vc = [sb("vc%d" % i, [128, 4, D], BF16) for i in range(2)]
            AT = sb("AT", [128, 4, TS], BF16)
            sm = [sb("sm%d" % i, [128, 4, 128], F32) for i in range(2)]
            Gm = [sb("Gm%d" % i, [128, NTT, 8, 512], BF16) for i in range(2)]
            WT = sb("WT", [128, 4, TS], BF16)
            acc = [sb("accD%d" % i, [128, D], F32) for i in range(NTT)]
            xt = sb("xtD", [128, D], F32)
            ssq = sb("ssD", [128, 1], F32)
            rstd = sb("rstdD", [128, 1], F32)
            pA = [self.ps(st, "pA%d" % i, [128, 512], F32) for i in range(2)]
            pE = [self.ps(st, "pE%d" % i, [128, 512], F32) for i in range(2)]
            pO = [self.ps(st, "pO%d" % i, [128, 512], F32) for i in range(2)]
            npa = npe = npo = nsm = 0
            nch = 0
            for sti in range(S // TS):
                t0 = sti * TS
                self.dma(h2s[:], self.h2T_d[:, :, t0:t0 + TS].rearrange("k p t -> p k t"),
                         r=[("h2T", sti * NTT + j) for j in range(NTT)], w=["h2s"])
                for hp in range(16):
                    pb = pA[npa % 2]
                    pk = "P:pA%d" % (npa % 2)
                    npa += 1
                    self.mms([(pb[:, 0:TS], wq[:, k, hp * 128:(hp + 1) * 128], h2s[:, k, :], k == 0, k == 7) for k in range(8)],
                             ["wq", "h2s"], [pk])
                    self.copy("act", qTs[:, hp, :], pb[:, 0:TS], [pk], ["qTsD"])
                for tt_ in range(NTT):
                    tsl = slice(tt_ * 128, (tt_ + 1) * 128)
                    sk = "s_sb%d" % tt_
                    for g in range(4):
                        pb = pA[npa % 2]
                        pk = "P:pA%d" % (npa % 2)
                        npa += 1
                        self.mms([(pb[:, q * 128:(q + 1) * 128], qTs[:, g * 4 + q, tsl], skT[:, g * 4 + q, :], True, True) for q in range(4)],
                                 ["qTsD", "skT"], [pk])
                        self.copy("act", s_sb[tt_][:, g * 4:(g + 1) * 4, :], pb[:].rearrange("p (q n) -> p q n", q=4), [pk], [sk])
                    for hp in range(16):
                        self.op("dve", lambda e, hp=hp, tt_=tt_: e.max(out=t16[:, hp, 0:8], in_=s_sb[tt_][:, hp, :]), [sk], ["t16"])
                        self.op("dve", lambda e, hp=hp, tt_=tt_: e.match_replace(out=mr[:], in_to_replace=t16[:, hp, 0:8],
                                                                               in_values=s_sb[tt_][:, hp, :], imm_value=NEG), [sk, "t16"], ["mrD"])
                        self.op("dve", lambda e, hp=hp: e.max(out=t16[:, hp, 8:16], in_=mr[:]), ["mrD"], ["t16"])
                    for h in range(8):
                        a0 = t16[:, 2 * h, :].rearrange("p (a o) -> p a o", o=1).to_broadcast([128, 16, 16])
                        a1 = t16[:, 2 * h + 1:2 * h + 2, :].to_broadcast([128, 16, 16])
                        self.tt("dve", cand[:].rearrange("p (a b) -> p a b", a=16), a0, a1, ALU.add, ["t16"], ["cand"])
                        self.op("dve", lambda e, h=h: e.max(out=c16[:, h, 0:8], in_=cand[:]), ["cand"], ["c16"])
                        self.op("dve", lambda e, h=h: e.match_replace(out=cand2[:], in_to_replace=c16[:, h, 0:8], in_values=cand[:], imm_value=NEG),
                                ["cand", "c16"], ["cand2"])
                        self.op("dve", lambda e, h=h: e.max(out=c16[:, h, 8:16], in_=cand2[:]), ["cand2"], ["c16"])
                    self.copy("dve", thr_t[tt_][:], c16[:, :, 15], ["c16"], ["thr_t%d" % tt_])
                    self.tt("dve", e16[:], c16[:], c16[:, :, 0:1].to_broadcast([128, 8, 16]), ALU.subtract, ["c16"], ["e16"])
                    self.act(e16[:], e16[:], AF.Exp, ["e16"], ["e16"])
                    self.op("dve", lambda e: e.tensor_reduce(out=Zs[:], in_=e16[:], axis=AX.X, op=ALU.add), ["e16"], ["Zs"])
                    self.act(Zs[:], Zs[:], AF.Ln, ["Zs"], ["Zs"])
                    self.stt(nb_t[tt_][:], c16[:, :, 0], -1.0, Zs[:], ALU.mult, ALU.subtract, ["c16", "Zs"], ["nb_t%d" % tt_])
                for c in range(NE // 512):
                    cb = nch % 2
                    nch += 1
                    self.dma(uTc[cb][:], self.uT_d[:, :, c * 512:(c + 1) * 512].rearrange("k d e -> d k e"), r=[("uT", c)], w=["uTc%d" % cb])
                    self.dma(vc[cb][:], self.vb_d[c * 512:(c + 1) * 512, :].rearrange("(a p) n -> p a n", p=128), r=[("vb", c)], w=["vc%d" % cb])
                    for a in range(4):
                        pb = pA[npa % 2]
                        pk = "P:pA%d" % (npa % 2)
                        npa += 1
                        self.mms([(pb[:, 0:TS], uTc[cb][:, k, a * 128:(a + 1) * 128], h2s[:, k, :], k == 0, k == 7) for k in range(8)],
                                 ["uTc%d" % cb, "h2s"], [pk])
                        self.act(AT[:, a, :], pb[:, 0:TS], AF.Gelu, [pk], ["AT"])
                    gmb = Gm[cb]
                    gk = "Gm%d" % cb
                    for tt_ in range(NTT):
                        for h in range(8):
                            smb = sm[nsm % 2]
                            smk = "sm%d" % (nsm % 2)
                            nsm += 1
                            peb = pE[npe % 2]
                            pek = "P:pE%d" % (npe % 2)
                            npe += 1
                            i0 = s_sb[tt_][:, 2 * h, c * 4:(c + 1) * 4].rearrange("p (a o) -> p a o", o=1).to_broadcast([128, 4, 128])
                            i1 = s_sb[tt_][:, 2 * h + 1:2 * h + 2, :].to_broadcast([128, 4, 128])
                            self.tt("pool", smb[:], i0, i1, ALU.add, ["s_sb%d" % tt_], [smk])
                            smf = smb[:].rearrange("p a j -> p (a j)")
                            self.act(peb[:], smf, AF.Exp, [smk, "nb_t%d" % tt_], [pek], bias=nb_t[tt_][:, h:h + 1])
                            self.stt(gmb[:, tt_, h, :], smf, thr_t[tt_][:, h:h + 1], peb[:], ALU.is_ge, ALU.mult,
                                     [smk, "thr_t%d" % tt_, pek], [gk])
                    for a in range(4):
                        pb = pA[npa % 2]
                        pk = "P:pA%d" % (npa % 2)
                        npa += 1
                        specs = []
                        for tt_ in range(NTT):
                            for h in range(8):
                                specs.append((pb[:, tt_ * 128:(tt_ + 1) * 128], gmb[:, tt_, h, a * 128:(a + 1) * 128], self.ident[:], h == 0, h == 7))
                        self.mms(specs, [gk, "ident"], [pk])
                        self.tt("dve", WT[:, a, :], pb[:, 0:TS], AT[:, a, :], ALU.mult, [pk, "AT"], ["WT"])
                    for tt_ in range(NTT):
                        for hf in range(2):
                            pob = pO[npo % 2]
                            pok = "P:pO%d" % (npo % 2)
                            npo += 1
                            self.mms([(pob[:], WT[:, a, tt_ * 128:(tt_ + 1) * 128], vc[cb][:, a, hf * 512:(hf + 1) * 512], a == 0, a == 3) for a in range(4)],
                                     ["WT", "vc%d" % cb], [pok])
                            hs = slice(hf * 512, (hf + 1) * 512)
                            if c == 0:
                                self.copy("dve", acc[tt_][:, hs], pob[:], [pok], ["accD%d" % tt_])
                            else:
                                self.tt("dve", acc[tt_][:, hs], pob[:], acc[tt_][:, hs], ALU.add, [pok, "accD%d" % tt_], ["accD%d" % tt_])
                for tt_ in range(NTT):
                    i = sti * NTT + tt_
                    sl = slice(i * 128, (i + 1) * 128)
                    self.dma(xt[:], self.xres_d[sl, :], r=[("xres", i)], w=["xtD"])
                    ak = "accD%d" % tt_
                    self.tt("dve", acc[tt_][:], acc[tt_][:], self.modb[:, 5 * D:6 * D], ALU.mult, [ak, "modb"], [ak])
                    self.tt("dve", xt[:], acc[tt_][:], xt[:], ALU.add, [ak, "xtD"], ["xtD"])
                    if not last:
                        self.dma(self.xres_d[sl, :], xt[:], r=["xtD"], w=[("xres", i)])
                    else:
                        junk = Gm[0][:, 0, 0:2, :].rearrange("p a b -> p (a b)")
                        self.act(junk, xt[:], AF.Square, ["xtD"], ["Gm0", "ssD"], accum_out=ssq[:])
                        self.act(ssq[:], ssq[:], AF.Sqrt, ["ssD"], ["ssD"], bias=1e-6, scale=1.0 / D)
                        self.op("dve", lambda e: e.reciprocal(out=rstd[:], in_=ssq[:]), ["ssD"], ["rstdD"])
                        self.stt(acc[tt_][:], xt[:], rstd[:], fnb[:], ALU.mult, ALU.mult, ["xtD", "rstdD", "fnb"], [ak])
                        self.dma(self.out_d[sl, :], acc[tt_][:], r=[ak], w=[("out", i)])
            self.barrier()


WEIGHTS = ["w_ada", "b_ada", "norm_mix", "w_in", "w_pool", "pool_scale", "w_out", "norm_ffn",
           "w_query", "sub_keys", "expert_u", "expert_v"]


def make_in_maps(inputs, S, nb):
    NT = S // 128
    maps = []
    for b in range(nb):
        m = {
            "x": np.ascontiguousarray(inputs["x"][b], dtype=np.float32),
            "c": np.ascontiguousarray(np.asarray(inputs["c"][b], dtype=np.float32).reshape(8, 128).T),
            "pos": np.ascontiguousarray(np.asarray(inputs["positions"][b], dtype=np.int32).reshape(NT, 128).T),
            "final_norm": np.ascontiguousarray(np.asarray(inputs["final_norm"], dtype=np.float32).reshape(1, D)),
        }
        for k in WEIGHTS:
            m[k] = np.ascontiguousarray(inputs[k], dtype=np.float32)
        maps.append(m)
    return maps


def kernel(**inputs):
    S = inputs["x"].shape[1]
    nb = inputs["x"].shape[0]
    kb = K(S)
    nc = kb.build()
    res = run_bass_kernel_spmd(nc, make_in_maps(inputs, S, nb), core_ids=list(range(nb)))
    return np.stack([np.asarray(r["out"], dtype=np.float32) for r in res.results], axis=0)
```
